# Optimizing a Trainium2 kernel written in Bass

```python
import math
import jax, jax.numpy as jnp
from jax import lax
import numpy as np


D_MODEL = 1024
BATCH = 32
SEQ = 2048
DEPTH = 1

HEAD_DIM = 64
RWKV_HEADS = 8
RWKV_WIDTH = RWKV_HEADS * HEAD_DIM
DECAY_LORA = 64
ICLR_LORA = 64
GATE_LORA = 128
GROUPNORM_EPS = 64e-5
ATTN_GROUPS = ((128, 1), (512, 4), (2048, 16))
HEADS_PER_GROUP = 4
ATTN_HEADS = HEADS_PER_GROUP * len(ATTN_GROUPS)
ATTN_WIDTH = ATTN_HEADS * HEAD_DIM
ATTN_OUT_WIDTH = HEADS_PER_GROUP * HEAD_DIM
ATTN_BLOCK = 128
N_BUCKETS = 32
MAX_DISTANCE = 2048
N_EXPERTS = 32
TOP_K = 4
D_EXPERT = D_MODEL
SWIGLU_ALPHA = 1.702
SWIGLU_LIMIT = 7.0
MOE_BLOCK = 512
NORM_EPS = 1e-6
NEG_INF = -1e30
IN_SPLITS = (RWKV_WIDTH, 2 * RWKV_WIDTH, 3 * RWKV_WIDTH,
             3 * RWKV_WIDTH + ATTN_WIDTH, 3 * RWKV_WIDTH + 2 * ATTN_WIDTH,
             3 * RWKV_WIDTH + 3 * ATTN_WIDTH, 3 * RWKV_WIDTH + 3 * ATTN_WIDTH + D_MODEL)
IN_COLS = IN_SPLITS[-1] + D_MODEL

kernel_name = 'hybrid_rwkv7_dilated_attn_moe_block'


def rms_norm(x, g):
    xf = x.astype(jnp.float32)
    y = xf * lax.rsqrt(jnp.mean(xf * xf, axis=-1, keepdims=True) + NORM_EPS)
    return (y * g).astype(x.dtype)


def token_shift(x):
    return jnp.pad(x, ((0, 0), (1, 0), (0, 0)))[:, :-1]


def wkv7_scan(r, decay, k, v, a_vec, b_vec):
    Bsz, S, H, N = r.shape
    xs = tuple(t.transpose(1, 0, 2, 3) for t in (r, decay, k, v, a_vec, b_vec))

    def step(state, inp):
        r_t, w_t, k_t, v_t, a_t, b_t = inp
        sa = jnp.einsum('bhvk,bhk->bhv', state, a_t)
        state = (state * w_t[:, :, None, :] + sa[..., None] * b_t[:, :, None, :]
                 + v_t[..., None] * k_t[:, :, None, :])
        return state, jnp.einsum('bhvk,bhk->bhv', state, r_t)

    _, ys = lax.scan(step, jnp.zeros((Bsz, H, N, N), jnp.float32), xs)
    return ys.transpose(1, 0, 2, 3)


def rwkv7_time_mix(h, r, k, v, mu_rkv, mu_wag, w0, w1, w2, a0, a1, a2, g1, g2,
                   k_k, k_a, r_k, ln_w, ln_b):
    Bsz, S, _ = h.shape
    f32 = jnp.float32
    h = h.astype(f32)
    r, k, v = r.astype(f32), k.astype(f32), v.astype(f32)
    dh = token_shift(h) - h
    xw, xa, xg = h + dh * mu_wag[0], h + dh * mu_wag[1], h + dh * mu_wag[2]
    r = r + (token_shift(r) - r) * mu_rkv[0]
    k = k + (token_shift(k) - k) * mu_rkv[1]
    v = v + (token_shift(v) - v) * mu_rkv[2]
    w_log = -jax.nn.softplus(-(w0 + jnp.tanh(xw @ w1) @ w2)) - 0.5
    decay = jnp.exp(-jnp.exp(w_log))
    a = jax.nn.sigmoid(a0 + (xa @ a1) @ a2)
    g = jax.nn.sigmoid(xg @ g1) @ g2
    hs = lambda t: t.reshape(Bsz, S, RWKV_HEADS, HEAD_DIM)
    kk = hs(k * k_k)
    kk = kk / jnp.maximum(jnp.linalg.norm(kk, axis=-1, keepdims=True), 1e-12)
    k = k * (1.0 + (a - 1.0) * k_a)
    rh, kh, vh, ah = hs(r), hs(k), hs(v), hs(a)
    y = wkv7_scan(rh, hs(decay), kh, vh, -kk, kk * ah)
    mu = jnp.mean(y, axis=-1, keepdims=True)
    var = jnp.mean(jnp.square(y - mu), axis=-1, keepdims=True)
    y = ((y - mu) * lax.rsqrt(var + GROUPNORM_EPS)).reshape(Bsz, S, RWKV_WIDTH) * ln_w + ln_b
    bonus = jnp.sum(rh * kh * r_k, axis=-1, keepdims=True) * vh
    y = (y + bonus.reshape(Bsz, S, RWKV_WIDTH)) * g
    return y


def t5_bucket(dist):
    max_exact = N_BUCKETS // 2
    large = max_exact + (jnp.log(jnp.maximum(dist, max_exact).astype(jnp.float32) / max_exact)
                         / math.log(MAX_DISTANCE / max_exact) * (N_BUCKETS - max_exact)).astype(jnp.int32)
    return jnp.where(dist < max_exact, dist, jnp.minimum(large, N_BUCKETS - 1))


def dilated_group_attention(q, k, v, bias_table, window, dilation):
    Bsz, S, H, Dh = q.shape
    L = S // dilation
    nb = -(-L // ATTN_BLOCK)
    Lp = nb * ATTN_BLOCK

    def to_blocks(t):
        t = t.reshape(Bsz, L, dilation, H, Dh).transpose(0, 2, 1, 3, 4)
        t = jnp.pad(t, ((0, 0), (0, 0), (0, Lp - L), (0, 0), (0, 0)))
        return t.reshape(Bsz, dilation, nb, ATTN_BLOCK, H, Dh)

    def with_prev(t):
        prev = jnp.pad(t, ((0, 0), (0, 0), (1, 0), (0, 0), (0, 0), (0, 0)))[:, :, :-1]
        return jnp.concatenate([prev, t], axis=3)

    def from_blocks(t):
        t = t.reshape((Bsz, dilation, Lp) + t.shape[4:])[:, :, :L]
        t = jnp.moveaxis(t, 1, 2)
        return t.reshape((Bsz, S) + t.shape[3:])

    qb = to_blocks(q)
    kw, vw = with_prev(to_blocks(k)), with_prev(to_blocks(v))
    qi = jnp.arange(ATTN_BLOCK)[:, None]
    kj = jnp.arange(2 * ATTN_BLOCK)[None, :]
    steps = qi - kj + ATTN_BLOCK
    bias = jnp.transpose(bias_table[t5_bucket(jnp.maximum(steps, 0) * dilation)], (2, 0, 1))
    band = (steps >= 0) & (steps <= window // dilation)
    valid = band[None] & ((jnp.arange(nb)[:, None, None] > 0) | (kj >= ATTN_BLOCK)[None])
    s = jnp.einsum('bznqhd,bznkhd->bznhqk', qb, kw) + bias.astype(jnp.float32)[None, None, None]
    s = jnp.where(valid[None, None, :, None], s, NEG_INF)
    m = jnp.max(s, axis=-1, keepdims=True)
    p = jnp.exp(s - m)
    den = jnp.sum(p, axis=-1, keepdims=True)
    o = jnp.einsum('bznhqk,bznkhd->bznqhd', p, vw) / jnp.transpose(den, (0, 1, 2, 4, 3, 5))
    lse = jnp.transpose((m + jnp.log(den))[..., 0], (0, 1, 2, 4, 3))
    return from_blocks(o), from_blocks(lse)


def dilated_attention(q, k, v, qn_g, kn_g, rel_bias):
    Bsz, S, _ = q.shape
    hs = lambda t: t.astype(jnp.float32).reshape(Bsz, S, ATTN_HEADS, HEAD_DIM)
    q = rms_norm(hs(q), qn_g) * (HEAD_DIM ** -0.5)
    k = rms_norm(hs(k), kn_g)
    v = hs(v)
    outs, lses = [], []
    for gi, (window, dilation) in enumerate(ATTN_GROUPS):
        sl = slice(gi * HEADS_PER_GROUP, (gi + 1) * HEADS_PER_GROUP)
        o, lse = dilated_group_attention(q[:, :, sl], k[:, :, sl], v[:, :, sl], rel_bias[:, sl],
                                         window, dilation)
        outs.append(o)
        lses.append(lse)
    alpha = jax.nn.softmax(jnp.stack(lses, axis=0), axis=0)
    o = jnp.sum(alpha[..., None] * jnp.stack(outs, axis=0), axis=0)
    return o.reshape(Bsz, S, ATTN_OUT_WIDTH)


def clamped_swiglu(u):
    x_glu, x_lin = u[..., ::2], u[..., 1::2]
    x_glu = jnp.minimum(x_glu, SWIGLU_LIMIT)
    x_lin = jnp.clip(x_lin, -SWIGLU_LIMIT, SWIGLU_LIMIT)
    return x_glu * jax.nn.sigmoid(SWIGLU_ALPHA * x_glu) * (x_lin + 1.0)


def moe_ffn(h, router_w, router_b, w1, b1, w2, b2):
    Bsz, S, D = h.shape
    N = Bsz * S
    hf = h.reshape(N, D)
    logits = (hf @ router_w + router_b).astype(jnp.float32)
    top_v, top_i = lax.top_k(logits, TOP_K)
    gates = jax.nn.softmax(top_v, axis=-1)
    A = N * TOP_K
    flat_e = top_i.reshape(A)
    flat_t = jnp.arange(A, dtype=jnp.int32) // TOP_K
    flat_g = gates.reshape(A)
    order = jnp.argsort(flat_e)
    se, st, sg = flat_e[order], flat_t[order], flat_g[order]
    counts = jnp.bincount(flat_e, length=N_EXPERTS)
    padded = (counts + MOE_BLOCK - 1) // MOE_BLOCK * MOE_BLOCK
    start = jnp.cumsum(counts) - counts
    pend = jnp.cumsum(padded)
    pstart = pend - padded
    dest = pstart[se] + jnp.arange(A, dtype=jnp.int32) - start[se]
    n_blocks = -(-A // MOE_BLOCK) + N_EXPERTS
    rows = n_blocks * MOE_BLOCK
    tok_buf = jnp.zeros((rows,), jnp.int32).at[dest].set(st)
    gate_buf = jnp.zeros((rows,), jnp.float32).at[dest].set(sg)
    blk_e = jnp.minimum(jnp.searchsorted(pend, jnp.arange(n_blocks, dtype=jnp.int32) * MOE_BLOCK,
                                         side='right'), N_EXPERTS - 1)

    def block(acc, inp):
        tok, g, e = inp
        u = hf[tok] @ w1[e] + b1[e]
        o = clamped_swiglu(u) @ w2[e] + b2[e]
        return acc.at[tok].add(o.astype(jnp.float32) * g[:, None]), None

    acc, _ = lax.scan(block, jnp.zeros((N, D), jnp.float32),
                      (tok_buf.reshape(n_blocks, MOE_BLOCK), gate_buf.reshape(n_blocks, MOE_BLOCK), blk_e))
    return acc.reshape(Bsz, S, D).astype(h.dtype)


def hybrid_layer(x, c, ada_w, ada_b, norm1_g, norm2_g, w_in, rwkv_mu_rkv, rwkv_mu_wag,
                 rwkv_w0, rwkv_w1, rwkv_w2, rwkv_a0, rwkv_a1, rwkv_a2, rwkv_g1, rwkv_g2,
                 rwkv_k_k, rwkv_k_a, rwkv_r_k, rwkv_ln_w, rwkv_ln_b, attn_qn_g, attn_kn_g,
                 rel_bias, w_br_rwkv, w_br_attn, w_out, router_w, router_b,
                 exp_w1, exp_b1, exp_w2, exp_b2):
    mod = jax.nn.silu(c) @ ada_w + ada_b
    shift1, scale1, gate1, shift2, scale2, gate2 = [m[:, None, :] for m in jnp.split(mod, 6, axis=-1)]
    h = rms_norm(x, norm1_g) * (1.0 + scale1) + shift1
    proj = h @ w_in
    r, k, v, qa, ka, va, gr, ga = jnp.split(proj, IN_SPLITS, axis=-1)
    y_rwkv = rwkv7_time_mix(h, r, k, v, rwkv_mu_rkv, rwkv_mu_wag, rwkv_w0, rwkv_w1, rwkv_w2,
                            rwkv_a0, rwkv_a1, rwkv_a2, rwkv_g1, rwkv_g2, rwkv_k_k, rwkv_k_a,
                            rwkv_r_k, rwkv_ln_w, rwkv_ln_b).astype(x.dtype) @ w_br_rwkv
    y_attn = dilated_attention(qa, ka, va, attn_qn_g, attn_kn_g, rel_bias).astype(x.dtype) @ w_br_attn
    mixed = jax.nn.sigmoid(gr) * y_rwkv + jax.nn.sigmoid(ga) * y_attn
    x = x + gate1 * (mixed @ w_out)
    h2 = rms_norm(x, norm2_g) * (1.0 + scale2) + shift2
    x = x + gate2 * moe_ffn(h2, router_w, router_b, exp_w1, exp_b1, exp_w2, exp_b2)
    return x


def setup_inputs(seed: int = 0) -> dict:
    key = jax.random.key(seed)
    ks = iter(jax.random.split(key, 40))
    f32 = jnp.float32
    nrm = lambda shape, scale: jax.random.normal(next(ks), shape, f32) * scale
    uni = lambda shape, lo, hi: jax.random.uniform(next(ks), shape, f32, lo, hi)
    L, D, RW = DEPTH, D_MODEL, RWKV_WIDTH
    return {
        'x': nrm((BATCH, SEQ, D), 1.0),
        'c': nrm((BATCH, D), 1.0),
        'ada_w': nrm((L, D, 6 * D), D ** -0.5),
        'ada_b': nrm((L, 6 * D), 0.02),
        'norm1_g': 1.0 + nrm((L, D), 0.02),
        'norm2_g': 1.0 + nrm((L, D), 0.02),
        'w_in': nrm((L, D, IN_COLS), D ** -0.5),
        'rwkv_mu_rkv': uni((L, 3, RW), 0.0, 1.0),
        'rwkv_mu_wag': uni((L, 3, D), 0.0, 1.0),
        'rwkv_w0': uni((L, RW), -6.0, 1.0),
        'rwkv_w1': nrm((L, D, DECAY_LORA), D ** -0.5),
        'rwkv_w2': nrm((L, DECAY_LORA, RW), DECAY_LORA ** -0.5),
        'rwkv_a0': nrm((L, RW), 0.1),
        'rwkv_a1': nrm((L, D, ICLR_LORA), D ** -0.5),
        'rwkv_a2': nrm((L, ICLR_LORA, RW), ICLR_LORA ** -0.5),
        'rwkv_g1': nrm((L, D, GATE_LORA), D ** -0.5),
        'rwkv_g2': nrm((L, GATE_LORA, RW), GATE_LORA ** -0.5),
        'rwkv_k_k': 0.85 + nrm((L, RW), 0.02),
        'rwkv_k_a': 1.0 + nrm((L, RW), 0.02),
        'rwkv_r_k': nrm((L, RWKV_HEADS, HEAD_DIM), 0.1),
        'rwkv_ln_w': 1.0 + nrm((L, RW), 0.02),
        'rwkv_ln_b': nrm((L, RW), 0.02),
        'attn_qn_g': 1.0 + nrm((L, HEAD_DIM), 0.02),
        'attn_kn_g': 1.0 + nrm((L, HEAD_DIM), 0.02),
        'rel_bias': nrm((N_BUCKETS, ATTN_HEADS), 0.5),
        'w_br_rwkv': nrm((L, RW, D), RW ** -0.5),
        'w_br_attn': nrm((L, ATTN_OUT_WIDTH, D), ATTN_OUT_WIDTH ** -0.5),
        'w_out': nrm((L, D, D), D ** -0.5),
        'router_w': nrm((L, D, N_EXPERTS), D ** -0.5),
        'router_b': nrm((L, N_EXPERTS), 0.01),
        'exp_w1': nrm((L, N_EXPERTS, D, 2 * D_EXPERT), D ** -0.5),
        'exp_b1': nrm((L, N_EXPERTS, 2 * D_EXPERT), 0.02),
        'exp_w2': nrm((L, N_EXPERTS, D_EXPERT, D), D_EXPERT ** -0.5),
        'exp_b2': nrm((L, N_EXPERTS, D), 0.02),
    }


def reference(x, c, ada_w, ada_b, norm1_g, norm2_g, w_in, rwkv_mu_rkv, rwkv_mu_wag,
              rwkv_w0, rwkv_w1, rwkv_w2, rwkv_a0, rwkv_a1, rwkv_a2, rwkv_g1, rwkv_g2,
              rwkv_k_k, rwkv_k_a, rwkv_r_k, rwkv_ln_w, rwkv_ln_b, attn_qn_g, attn_kn_g,
              rel_bias, w_br_rwkv, w_br_attn, w_out, router_w, router_b,
              exp_w1, exp_b1, exp_w2, exp_b2):
    for l in range(DEPTH):
        x = hybrid_layer(x, c, ada_w[l], ada_b[l], norm1_g[l], norm2_g[l], w_in[l],
                         rwkv_mu_rkv[l], rwkv_mu_wag[l], rwkv_w0[l], rwkv_w1[l], rwkv_w2[l],
                         rwkv_a0[l], rwkv_a1[l], rwkv_a2[l], rwkv_g1[l], rwkv_g2[l],
                         rwkv_k_k[l], rwkv_k_a[l], rwkv_r_k[l], rwkv_ln_w[l], rwkv_ln_b[l],
                         attn_qn_g[l], attn_kn_g[l], rel_bias, w_br_rwkv[l], w_br_attn[l],
                         w_out[l], router_w[l], router_b[l], exp_w1[l], exp_b1[l],
                         exp_w2[l], exp_b2[l])
    return x
```

```python
import math
from contextlib import ExitStack

import numpy as np
import concourse.bass as bass
import concourse.mybir as mybir
from concourse.bass_utils import run_bass_kernel_spmd

F32 = mybir.dt.float32
BF16 = mybir.dt.bfloat16
ALU = mybir.AluOpType
AF = mybir.ActivationFunctionType
AX = mybir.AxisListType

T = 2048
D = 1024
NCORES = 8
NBFULL = 4
INC = 5888
SEM_LIMIT = 30000
N_DMA_SEMS = 8
ATTN_GROUPS = ((128, 1), (512, 4), (2048, 16))
NEG = -1.0e30


class Dep:
    __slots__ = ("w", "r", "excl")

    def __init__(self, excl=False):
        self.w = None
        self.r = []
        self.excl = excl


class Buf:
    def __init__(self, t, nslots=1, excl=False):
        self.t = t
        self.d = [Dep(excl) for _ in range(nslots)]

    def __getitem__(self, k):
        return self.t[k]


class Sched:
    ENG = ("pe", "act", "dve", "pool", "sp")

    def __init__(self, nc, stack):
        self.nc = nc
        self.stack = stack
        self.q = {e: [] for e in self.ENG}
        self.cnt = {e: 0 for e in self.ENG}
        self.sem = {e: stack.enter_context(nc.semaphore(f"s_{e}")) for e in self.ENG if e != "sp"}
        self.seen = {e: {} for e in self.ENG}
        self.dsem, self.dcnt, self.dnext = {}, {}, {}
        for e in ("sp", "pool", "act"):
            self.dsem[e] = [stack.enter_context(nc.semaphore(f"d_{e}{i}")) for i in range(N_DMA_SEMS)]
            self.dcnt[e] = [0] * N_DMA_SEMS
            self.dnext[e] = 0
        self.nrot = 0
        self.old = []

    def _need(self, eng, toks):
        waits = {}
        for tok in toks:
            if tok is None:
                continue
            s, v, src = tok
            if src == eng and eng == "pe":
                continue
            key = id(s)
            if self.seen[eng].get(key, 0) >= v:
                continue
            if key not in waits or waits[key][1] < v:
                waits[key] = (s, v)
        for key, (s, v) in waits.items():
            self.seen[eng][key] = v
        return list(waits.values())

    @staticmethod
    def _deps(reads, writes, eng=None):
        toks = []
        for d in reads:
            toks.append(d.w)
            if d.excl:
                toks.extend(t_ for t_ in d.r if t_[2] != eng)
        for d in writes:
            toks.append(d.w)
            toks.extend(d.r)
        return toks

    @staticmethod
    def _commit(tok, reads, writes):
        for d in reads:
            d.r.append(tok)
            if len(d.r) > 48:
                d.r = d.r[-48:]
        for d in writes:
            d.w = tok
            d.r = []

    def _rotate(self, eng):
        if self.cnt[eng] >= SEM_LIMIT:
            self.old.append((self.sem[eng], self.cnt[eng], eng))
            self.sem[eng] = self.stack.enter_context(self.nc.semaphore(f"s_{eng}_r{self.nrot}"))
            self.nrot += 1
            self.cnt[eng] = 0

    def op(self, eng, fn, reads=(), writes=()):
        reads, writes = list(reads), list(writes)
        self._rotate(eng)
        waits = self._need(eng, self._deps(reads, writes, eng))
        self.cnt[eng] += 1
        sem = self.sem[eng]
        tok = (sem, self.cnt[eng], eng)

        def thunk(e, fn=fn, waits=waits, sem=sem):
            for s, v in waits:
                e.wait_ge(s, v)
            fn(e).then_inc(sem, 1)

        self.q[eng].append(thunk)
        self._commit(tok, reads, writes)
        return tok

    def dma(self, eng, out, in_, reads=(), writes=(), **kw):
        reads, writes = list(reads), list(writes)
        i = self.dnext[eng]
        self.dnext[eng] = (i + 1) % N_DMA_SEMS
        sem = self.dsem[eng][i]
        prev = self.dcnt[eng][i]
        toks = self._deps(reads, writes)
        if prev > 0:
            toks.append((sem, prev, "dma"))
        waits = self._need(eng, toks)
        self.dcnt[eng][i] = prev + 16
        tok = (sem, prev + 16, "dma")

        def thunk(e, waits=waits, sem=sem, out=out, in_=in_, kw=kw):
            for s, v in waits:
                e.wait_ge(s, v)
            e.dma_start(out=out, in_=in_, **kw).then_inc(sem, 16)

        self.q[eng].append(thunk)
        self._commit(tok, reads, writes)
        return tok

    def barrier(self):
        toks = []
        for x in self.ENG:
            if x != "sp" and self.cnt[x] > 0:
                toks.append((self.sem[x], self.cnt[x], x + "_b"))
        for (s, v, x) in self.old:
            toks.append((s, v, x + "_b"))
        for qn in self.dsem:
            for s, v in zip(self.dsem[qn], self.dcnt[qn]):
                if v > 0:
                    toks.append((s, v, "dma"))
        for e in self.ENG:
            waits = self._need(e, toks)

            def thunk(en, waits=waits):
                for s, v in waits:
                    en.wait_ge(s, v)

            self.q[e].append(thunk)

    def emit(self):
        nc = self.nc
        q = self.q
        with nc.Block() as block:
            @block.tensor
            def _(e):
                for th in q["pe"]:
                    th(e)

            @block.scalar
            def _(e):
                for th in q["act"]:
                    th(e)

            @block.vector
            def _(e):
                for th in q["dve"]:
                    th(e)

            @block.gpsimd
            def _(e):
                for th in q["pool"]:
                    th(e)

            @block.sync
            def _(e):
                for th in q["sp"]:
                    th(e)
        self.q = {e: [] for e in self.ENG}


def build(NB=NBFULL, stages="0ABCDE", debug=False, ext_in=(), lim=None):
    NTOK = NB * T
    NT = NTOK // 128
    NTL = NT if lim is None else min(NT, lim)
    nc = bass.Bass("TRN2", target_bir_lowering=False)

    in_names = []

    def din(name, shape, dt=F32):
        if name in ("exp_w1", "b1fm", "exp_w2", "exp_b2") and "E" not in stages:
            return None
        in_names.append(name)
        return nc.dram_tensor(name, list(shape), dt, kind="ExternalInput").ap()

    def dscr(name, shape, dt=F32):
        if name in ext_in:
            kind = "ExternalInput"
        elif debug:
            kind = "ExternalOutput"
        else:
            kind = "Internal"
        return nc.dram_tensor(name, list(shape), dt, kind=kind).ap()

    x_d = din("x", [NTOK, D])
    cT_d = din("cT", [128, 8, NB])
    adaw_d = din("ada_w", [D, 6 * D])
    adab_d = din("ada_b", [1, 6 * D])
    n1g_d = din("n1g", [128, 8])
    n2g_d = din("n2g", [128, 8])
    win_d = din("w_in", [D, INC])
    crow_d = din("crow", [1, 5120])
    muwag_d = din("muwag", [128, 3, 8])
    lw1_d = din("lw1", [D, 256])
    l2_d = din("l2", [128, 1536])
    qkg_d = din("qkg", [1, 512])
    biasT_d = din("biasT", [128, 3, 4, 2, 128])
    maskT_d = din("maskT", [128, 2, 128])
    wbrr_d = din("w_br_rwkv", [512, D])
    wbra_d = din("w_br_attn", [256, D])
    wout_d = din("w_out", [D, D])
    rw_d = din("router_w", [D, 32])
    rb_d = din("router_b", [1, 32])
    ew1_d = din("exp_w1", [32, D, 2 * D])
    b1fm_d = din("b1fm", [128, 32, 2, 8])
    ew2_d = din("exp_w2", [32, D, D])
    eb2_d = din("exp_b2", [32, D])
    ident_d = din("ident", [128, 128])
    tri_d = din("tri", [128, 3, 128])

    out_d = nc.dram_tensor("out", [NTOK, D], F32, kind="ExternalOutput").ap()

    modrow_s = dscr("modrow", [NB, 6 * D])
    proj_s = dscr("proj", [NTOK, INC])
    lora_s = dscr("lora", [NTOK, 1536])
    rwo_s = dscr("rwo", [NTOK, 512])
    ao_s = dscr("ao", [3, NTOK, 260])
    x1_s = dscr("x1", [NTOK, D])
    h2T_s = dscr("h2T", [128, 8, NTOK], BF16)
    GT_s = dscr("GT", [32, NTOK])
    Gtm_s = dscr("Gtm", [NTOK, 32])

    with ExitStack() as top:
        S = Sched(nc, top)

        def tt(eng, out, in0, in1, op, R, W):
            return S.op(eng, lambda e: e.tensor_tensor(out, in0, in1, op), R, W)

        def ts(eng, out, in0, s1, s2, op0, op1, R, W):
            if op1 is None:
                return S.op(eng, lambda e: e.tensor_scalar(out, in0, s1, None, op0=op0), R, W)
            return S.op(eng, lambda e: e.tensor_scalar(out, in0, s1, s2, op0=op0, op1=op1), R, W)

        def stt(eng, out, in0, sc, in1, op0, op1, R, W):
            return S.op(eng, lambda e: e.scalar_tensor_tensor(out, in0, sc, in1, op0=op0, op1=op1), R, W)

        def act(out, in_, func, R, W, **kw):
            return S.op("act", lambda e: e.activation(out, in_, func, **kw), R, W)

        def cp(eng, out, in_, R, W):
            if eng == "act":
                return S.op("act", lambda e: e.copy(out, in_), R, W)
            return S.op(eng, lambda e: e.tensor_copy(out, in_), R, W)

        def mm(out, lhsT, rhs, start, stop, R, W):
            return S.op("pe", lambda e: e.matmul(out, lhsT=lhsT, rhs=rhs, start=start, stop=stop), R, W)

        def tr(out, in_, idn, R, W):
            return S.op("pe", lambda e: e.transpose(out, in_, idn), R, W)

        def red(eng, out, in_, R, W, op=ALU.add):
            return S.op(eng, lambda e: e.tensor_reduce(out, in_, axis=AX.X, op=op), R, W)

        def recip(out, in_, R, W):
            return S.op("dve", lambda e: e.reciprocal(out, in_), R, W)

        def mset(eng, ap, val, W):
            return S.op(eng, lambda e: e.memset(ap, val), [], W)

        class Pool_:
            def __init__(self, st):
                self.st = st

            def sb(self, name, shape, dt=F32, n=1):
                return Buf(self.st.enter_context(nc.sbuf_tensor("sb_" + name, list(shape), dt)), n)

            def ps(self, name, shape, dt=F32, n=1):
                return Buf(self.st.enter_context(nc.psum_tensor("ps_" + name, list(shape), dt)), n, excl=True)

            def ring(self, name, shape, dt=F32, k=2, psum=False):
                f = self.ps if psum else self.sb
                return [f(f"{name}{i}", shape, dt) for i in range(k)]

        G0 = Pool_(top)
        ident = G0.sb("ident", [128, 128])
        identb = G0.sb("identb", [128, 128], BF16)
        sc1 = G0.sb("sc1", [128, 8, NB])
        sh1 = G0.sb("sh1", [128, 8, NB])
        sc2 = G0.sb("sc2", [128, 8, NB])
        sh2 = G0.sb("sh2", [128, 8, NB])

        def phase0():
            with ExitStack() as ph:
                P = Pool_(ph)
                S.dma("sp", ident[:], ident_d[:, :], writes=ident.d)
                cp("dve", identb[:], ident[:], ident.d, identb.d)
                cT = P.sb("cT", [128, 8, NB])
                scT = P.sb("scT", [128, 8, NB])
                S.dma("sp", cT[:], cT_d[:, :, :], writes=cT.d)
                act(scT[:], cT[:], AF.Silu, cT.d, scT.d)
                adab = P.sb("adab", [NB, 6 * D])
                S.dma("sp", adab[:], adab_d.partition_broadcast(NB), writes=adab.d)
                modsb = P.sb("modsb", [NB, 6 * D])
                aw = P.ring("aw", [128, 3072], k=2)
                pm = [P.ps(f"pm{n}", [NB, 512]) for n in range(6)]
                it = 0
                for half in range(2):
                    for kc in range(8):
                        a = aw[it % 2]
                        it += 1
                        S.dma("sp", a[:], adaw_d[kc * 128:(kc + 1) * 128, half * 3072:(half + 1) * 3072], writes=a.d)
                        for n in range(6):
                            mm(pm[n][:], scT[:, kc, :], a[:, n * 512:(n + 1) * 512], kc == 0, kc == 7,
                               scT.d + a.d, pm[n].d)
                    for n in range(6):
                        c0 = half * 3072 + n * 512
                        tt("dve", modsb[:, c0:c0 + 512], pm[n][:], adab[:, c0:c0 + 512], ALU.add,
                           pm[n].d + adab.d, modsb.d)
                S.dma("sp", modrow_s[:, :], modsb[:], reads=modsb.d)
                pT_ = P.ps("pT", [128, 512])
                pT = pT_[:, 0:48 * NB].rearrange("p (j b) -> p j b", b=NB)
                for j in range(48):
                    tr(pT[:, j, :], modsb[0:NB, j * 128:(j + 1) * 128], ident[0:NB, 0:NB], modsb.d + ident.d, pT_.d)
                modT = P.sb("modT", [128, 48, NB])
                cp("dve", modT[:], pT, pT_.d, modT.d)
                n1g = P.sb("n1g", [128, 8])
                n2g = P.sb("n2g", [128, 8])
                S.dma("sp", n1g[:], n1g_d[:, :], writes=n1g.d)
                S.dma("sp", n2g[:], n2g_d[:, :], writes=n2g.d)
                tmp = P.sb("tmpm", [128, 8, NB])
                for (sc, sh, g, s_sh, s_sc) in ((sc1, sh1, n1g, 0, 8), (sc2, sh2, n2g, 24, 32)):
                    ts("dve", tmp[:], modT[:, s_sc:s_sc + 8, :], 1.0, None, ALU.add, None, modT.d, tmp.d)
                    tt("dve", sc[:], tmp[:], g[:].unsqueeze(2).to_broadcast([128, 8, NB]), ALU.mult,
                       tmp.d + g.d, sc.d)
                    cp("dve", sh[:], modT[:, s_sh:s_sh + 8, :], modT.d, sh.d)
                S.barrier()
                S.emit()

        def phaseA():
            with ExitStack() as ph:
                P = Pool_(ph)
                winb = P.sb("winb", [128, 8, INC], BF16, n=8)
                for c in range(8):
                    S.dma("pool", winb[:, c, :], win_d[c * 128:(c + 1) * 128, :], writes=[winb.d[c]])
                lw1f = P.sb("lw1f", [128, 8, 256])
                S.dma("sp", lw1f[:], lw1_d.rearrange("(c p) n -> p c n", p=128), writes=lw1f.d)
                muw = P.sb("muw", [128, 3, 8])
                S.dma("sp", muw[:], muwag_d[:, :, :], writes=muw.d)
                lw1b = P.sb("lw1b", [128, 8, 256], BF16)
                lw1m = P.sb("lw1m", [128, 8, 256], BF16)
                cp("dve", lw1b[:], lw1f[:], lw1f.d, lw1b.d)
                for k, (a, b) in enumerate(((0, 64), (64, 128), (128, 256))):
                    tt("dve", lw1m[:, :, a:b], lw1f[:, :, a:b],
                       muw[:, k, :].unsqueeze(2).to_broadcast([128, 8, b - a]), ALU.mult,
                       lw1f.d + muw.d, lw1m.d)
                l2b = P.sb("l2b", [128, 1536], BF16)
                S.dma("pool", l2b[:], l2_d[:, :], writes=l2b.d)

                xt = P.ring("xt", [128, D], k=2)
                xn = P.ring("xn", [128, D], k=2)
                junk = P.sb("junk", [128, D])
                ss = P.ring("ss", [128, 1], k=2)
                hTf = [P.sb(f"hTf{k}", [128, 8, 129], n=8) for k in range(2)]
                hTb = P.ring("hTb", [128, 8, 128], BF16, k=2)
                dhb = P.ring("dhb", [128, 8, 128], BF16, k=2)
                ptr = P.ps("ptr", [128, 8, 128], n=2)
                pp = P.ring("pp", [128, 512], k=3, psum=True)
                pl = P.ps("pl", [128, 4, 128])
                pl2 = P.ring("pl2_", [128, 512], k=2, psum=True)
                stg = P.ring("stg", [128, 2048], k=3)
                l1sb = P.sb("l1sb", [128, 128], BF16)
                l1g = P.sb("l1g", [128, 128], BF16)
                lst = P.ring("lst", [128, 1536], k=2)

                def stage1a(i):
                    x_ = xt[i % 2]
                    xn_ = xn[i % 2]
                    s_ = ss[i % 2]
                    S.dma("sp", x_[:], x_d[i * 128:(i + 1) * 128, :], writes=x_.d)
                    act(junk[:], x_[:], AF.Square, x_.d, junk.d + s_.d, accum_out=s_[:])
                    act(s_[:], s_[:], AF.Sqrt, s_.d, s_.d, bias=1e-6, scale=1.0 / D)
                    recip(s_[:], s_[:], s_.d, s_.d)
                    ts("dve", xn_[:], x_[:], s_[:, 0:1], None, ALU.mult, None, x_.d + s_.d, xn_.d)

                def stage1b(i):
                    b = i // 16
                    xn_ = xn[i % 2]
                    cur = hTf[i % 2]
                    nxt = hTf[(i + 1) % 2]
                    for c in range(8):
                        tr(ptr[:, c, :], xn_[:, c * 128:(c + 1) * 128], ident[:], xn_.d + ident.d, [ptr.d[c // 4]])
                    if i % 16 == 0:
                        mset("pool", cur[:, :, 0:1], 0.0, cur.d)
                    for c in range(8):
                        if c < 4:
                            act(cur[:, c, 1:129], ptr[:, c, :], AF.Identity, [ptr.d[c // 4]] + sc1.d + sh1.d, [cur.d[c]],
                                bias=sh1[:, c, b:b + 1], scale=sc1[:, c, b:b + 1])
                        else:
                            ts("dve", cur[:, c, 1:129], ptr[:, c, :], sc1[:, c, b:b + 1], sh1[:, c, b:b + 1],
                               ALU.mult, ALU.add, [ptr.d[c // 4]] + sc1.d + sh1.d, [cur.d[c]])
                    cp("pool", hTb[i % 2][:], cur[:, :, 1:129], cur.d, hTb[i % 2].d)
                    tt("pool", dhb[i % 2][:], cur[:, :, 0:128], cur[:, :, 1:129], ALU.subtract, cur.d, dhb[i % 2].d)
                    cp("pool", nxt[:, :, 0:1], cur[:, :, 128:129], cur.d, nxt.d)

                nev = [0]

                def stage2(i, mid):
                    hb = hTb[i % 2]
                    db = dhb[i % 2]
                    for half in range(2):
                        k = 0
                        for c in range(8):
                            for (wsrc, rsrc) in ((lw1b, hb), (lw1m, db)):
                                mm(pl[:, half, :], wsrc[:, c, half * 128:(half + 1) * 128], rsrc[:, c, :],
                                   k == 0, k == 15, wsrc.d + rsrc.d, pl.d)
                                k += 1
                    act(l1sb[0:64, :], pl[0:64, 0, :], AF.Tanh, pl.d, l1sb.d)
                    cp("dve", l1sb[64:128, :], pl[64:128, 0, :], pl.d, l1sb.d)
                    act(l1g[:], pl[:, 1, :], AF.Sigmoid, pl.d, l1g.d)
                    for n in range(12):
                        if n == 5:
                            mid()
                        w = 512 if n < 11 else 256
                        p_ = pp[n % 3]
                        for c in range(8):
                            mm(p_[:, 0:w], hb[:, c, :], winb[:, c, n * 512:n * 512 + w], c == 0, c == 7,
                               hb.d + [winb.d[c]], p_.d)
                        sg_ = stg[n // 4]
                        col = (n % 4) * 512
                        if n % 2 == 0:
                            cp("act", sg_[:, col:col + w], p_[:, 0:w], p_.d, sg_.d)
                        else:
                            cp("dve", sg_[:, col:col + w], p_[:, 0:w], p_.d, sg_.d)
                        if n % 4 == 3 or n == 11:
                            g0 = (n // 4) * 2048
                            wid = 2048 if n < 11 else 1792
                            S.dma("sp", proj_s[i * 128:(i + 1) * 128, g0:g0 + wid], sg_[:, 0:wid], reads=sg_.d)
                    ls = lst[i % 2]
                    mm(pl2[0][:], l1sb[0:64, :], l2b[0:64, 0:512], True, True, l1sb.d + l2b.d, pl2[0].d)
                    mm(pl2[1][:], l1sb[64:128, :], l2b[64:128, 512:1024], True, True, l1sb.d + l2b.d, pl2[1].d)
                    cp("act", ls[:, 0:512], pl2[0][:], pl2[0].d, ls.d)
                    cp("dve", ls[:, 512:1024], pl2[1][:], pl2[1].d, ls.d)
                    mm(pl2[0][:], l1g[:], l2b[:, 1024:1536], True, True, l1g.d + l2b.d, pl2[0].d)
                    cp("act", ls[:, 1024:1536], pl2[0][:], pl2[0].d, ls.d)
                    S.dma("sp", lora_s[i * 128:(i + 1) * 128, :], ls[:], reads=ls.d)

                stage1a(0)
                stage1b(0)
                for i in range(NTL):
                    if i + 1 < NTL:
                        stage1a(i + 1)
                        stage2(i, lambda i=i: stage1b(i + 1))
                    else:
                        stage2(i, lambda: None)
                S.barrier()
                S.emit()

        def phaseB():
            with ExitStack() as ph:
                P = Pool_(ph)
                crow = P.sb("crow", [128, 5120])
                S.dma("sp", crow[:], crow_d.partition_broadcast(128), writes=crow.d)
                MU, W0, A0, KK_, KA, RK, LNW, LNB = (crow[:, 0:1536], crow[:, 1536:2048], crow[:, 2048:2560],
                                                      crow[:, 2560:3072], crow[:, 3072:3584], crow[:, 3584:4096],
                                                      crow[:, 4096:4608], crow[:, 4608:5120])
                tri = P.sb("tri", [128, 3, 128])
                S.dma("sp", tri[:], tri_d[:, :, :], writes=tri.d)
                mG = P.sb("mG", [128, 3, 128])
                mQ = P.sb("mQ", [128, 2, 128])
                cp("dve", mG[:, 0, :], tri[:, 0, :], tri.d, mG.d)
                cp("dve", mG[:, 1, :], tri[:, 1, :], tri.d, mG.d)
                cp("dve", mG[:, 2, :], tri[:, 0, :], tri.d, mG.d)
                cp("dve", mQ[:, 0, :], tri[:, 2, :], tri.d, mQ.d)
                cp("dve", mQ[:, 1, :], tri[:, 1, :], tri.d, mQ.d)
                ones = P.sb("ones", [128, 1])
                mset("dve", ones[:], 1.0, ones.d)

                cur = P.ring("cur", [128, 1536], k=2)
                prv = P.ring("prv", [128, 1536], k=2)
                lo = P.ring("lo", [128, 1536], k=2)
                mix = P.sb("mix", [128, 1536])
                dd = P.sb("dd", [128, 1536])
                logwr = P.ring("logw", [128, 512], k=2)
                asig = P.sb("asig", [128, 512])
                kk = P.sb("kk", [128, 512])
                t5 = P.sb("t5", [128, 512])
                t6 = P.sb("t6", [128, 512])
                kmod = P.sb("kmod", [128, 512])
                s8 = P.sb("s8", [128, 8])
                s8b = P.sb("s8b", [128, 8])
                epos = P.sb("epos", [128, 512])
                eneg = P.sb("eneg", [128, 512])
                eex = P.sb("eex", [128, 512])
                ARBKr = P.ring("ARBK", [128, 4, 512], k=2)
                bonusr = P.ring("bonus", [128, 512], k=2)
                vmixr = P.ring("vmix", [128, 512], k=2)
                ptr4 = P.ring("ptr4_", [64, 4, 128], k=2, psum=True)
                ART = P.sb("ART", [64, 8, 4, 128], n=8)
                pG = P.ps("pG", [128, 512])
                pS = P.ring("pS", [128, 4, 128], k=2, psum=True)
                Gs = P.sb("Gs", [128, 8, 3, 128], n=8)
                Sb = [P.sb(f"Sb{k}", [128, 8, 3, 128], n=8) for k in range(2)]
                pX = P.ps("pX", [128, 8, 64])
                pY = P.ps("pY", [128, 512])
                pcum = pY
                pZ = P.ps("pZ", [64, 8, 64])
                Xs = P.sb("Xs", [128, 8, 64], n=8)
                Us = P.sb("Us", [128, 8, 64], n=8)
                Z = [[P.sb(f"Z{b}_{k}", [64, 8, 64], n=8) for k in range(2)] for b in range(NB)]
                WC = P.sb("WC", [64, 8], n=8)
                y = P.sb("y", [128, 512])
                yc = P.sb("yc", [128, 512])
                rwo = P.ring("rwo", [128, 512], k=2)
                for b in range(NB):
                    mset("pool", Z[b][0][:], 0.0, Z[b][0].d)

                h3 = lambda ap: ap.rearrange("p (h d) -> p h d", h=8)
                b8 = lambda ap: ap.unsqueeze(2).to_broadcast([128, 8, 64])
                EM05 = -math.exp(-0.5)
                it = [0]

                def loadB(idx, b, c):
                    k_ = idx % 2
                    n0 = b * T + c * 128
                    cu, pv, lo_ = cur[k_], prv[k_], lo[k_]
                    S.dma("sp", cu[:], proj_s[n0:n0 + 128, 0:1536], writes=cu.d)
                    if c == 0:
                        mset("pool", pv[0:1, :], 0.0, pv.d)
                        S.dma("sp", pv[1:128, :], proj_s[n0:n0 + 127, 0:1536], writes=pv.d)
                    else:
                        S.dma("sp", pv[:], proj_s[n0 - 1:n0 + 127, 0:1536], writes=pv.d)
                    S.dma("sp", lo_[:], lora_s[n0:n0 + 128, :], writes=lo_.d)

                def pre_gen(idx, b, c):
                    k_ = idx % 2
                    cu, pv, lo_ = cur[k_], prv[k_], lo[k_]
                    ARBK, logw, vmix, bonus = ARBKr[k_], logwr[k_], vmixr[k_], bonusr[k_]
                    tt("dve", dd[:], pv[:], cu[:], ALU.subtract, pv.d + cu.d, dd.d)
                    yield
                    tt("pool", dd[:], dd[:], MU, ALU.mult, dd.d + crow.d, dd.d)
                    yield
                    tt("dve", mix[:], cu[:], dd[:], ALU.add, cu.d + dd.d, mix.d)
                    yield
                    rm, km, vm = mix[:, 0:512], mix[:, 512:1024], mix[:, 1024:1536]
                    tt("dve", t5[:], lo_[:, 0:512], W0, ALU.add, lo_.d + crow.d, t5.d)
                    yield
                    act(t5[:], t5[:], AF.Sigmoid, t5.d, t5.d)
                    yield
                    ts("pool", logw[:], t5[:], EM05, None, ALU.mult, None, t5.d, logw.d)
                    yield
                    tt("dve", t6[:], lo_[:, 512:1024], A0, ALU.add, lo_.d + crow.d, t6.d)
                    yield
                    act(asig[:], t6[:], AF.Sigmoid, t6.d, asig.d)
                    yield
                    tt("pool", kk[:], km, KK_, ALU.mult, mix.d + crow.d, kk.d)
                    yield
                    tt("pool", t6[:], kk[:], kk[:], ALU.mult, kk.d, t6.d)
                    yield
                    red("dve", s8[:], h3(t6[:]), t6.d, s8.d)
                    yield
                    act(s8[:], s8[:], AF.Sqrt, s8.d, s8.d)
                    yield
                    ts("dve", s8[:], s8[:], 1e-12, None, ALU.max, None, s8.d, s8.d)
                    yield
                    recip(s8[:], s8[:], s8.d, s8.d)
                    yield
                    tt("dve", h3(kk[:]), h3(kk[:]), b8(s8[:]), ALU.mult, kk.d + s8.d, kk.d)
                    yield
                    stt("dve", t6[:], asig[:], -1.0, KA, ALU.add, ALU.mult, asig.d + crow.d, t6.d)
                    yield
                    stt("dve", kmod[:], t6[:], 1.0, km, ALU.add, ALU.mult, t6.d + mix.d, kmod.d)
                    yield
                    mm(pcum[:], tri[:, 0, :], logw[:], True, True, tri.d + logw.d, pcum.d)
                    yield
                    act(epos[:], pcum[:], AF.Exp, pcum.d, epos.d)
                    yield
                    act(eneg[:], pcum[:], AF.Exp, pcum.d, eneg.d, scale=-1.0)
                    yield
                    tt("dve", t5[:], pcum[:], logw[:], ALU.subtract, pcum.d + logw.d, t5.d)
                    yield
                    act(eex[:], t5[:], AF.Exp, t5.d, eex.d)
                    yield
                    stt("dve", ARBK[:, 0, :], kk[:], -1.0, eex[:], ALU.mult, ALU.mult, kk.d + eex.d, ARBK.d)
                    yield
                    tt("pool", ARBK[:, 1, :], rm, epos[:], ALU.mult, mix.d + epos.d, ARBK.d)
                    yield
                    tt("pool", t5[:], kk[:], asig[:], ALU.mult, kk.d + asig.d, t5.d)
                    yield
                    tt("pool", ARBK[:, 2, :], t5[:], eneg[:], ALU.mult, t5.d + eneg.d, ARBK.d)
                    yield
                    tt("dve", ARBK[:, 3, :], kmod[:], eneg[:], ALU.mult, kmod.d + eneg.d, ARBK.d)
                    yield
                    tt("pool", t6[:], rm, kmod[:], ALU.mult, mix.d + kmod.d, t6.d)
                    yield
                    tt("pool", t6[:], t6[:], RK, ALU.mult, t6.d + crow.d, t6.d)
                    yield
                    red("dve", s8b[:], h3(t6[:]), t6.d, s8b.d)
                    yield
                    tt("dve", h3(bonus[:]), h3(vm), b8(s8b[:]), ALU.mult, mix.d + s8b.d, bonus.d)
                    yield
                    cp("pool", vmix[:], vm, mix.d, vmix.d)
                    yield


                def chunk(idx, b, c, drip):
                    k_ = idx % 2
                    n0 = b * T + c * 128
                    cu, pv, lo_ = cur[k_], prv[k_], lo[k_]
                    ARBK, logw, vmix, bonus = ARBKr[k_], logwr[k_], vmixr[k_], bonusr[k_]

                    S0 = Sb[0]
                    for h in range(8):
                        pt_ = ptr4[h % 2]
                        hs = slice(h * 64, (h + 1) * 64)
                        for q in range(4):
                            tr(pt_[:, q, :], ARBK[:, q, hs], ident[:], ARBK.d + ident.d, pt_.d)
                        cp("act", ART[:, h, :, :], pt_[:], pt_.d, [ART.d[h]])
                        AT, RT, BT, KT = (ART[:, h, q, :] for q in range(4))
                        AR = ART[:, h, 0:2, :]
                        ad = [ART.d[h]]
                        mm(pG[:, 0:128], BT, RT, True, True, ad, pG.d)
                        mm(pG[:, 128:384], KT, AR, True, True, ad, pG.d)
                        tt("dve", Gs[:, h, :, :], pG[:, 0:384].rearrange("p (a b) -> p a b", a=3), mG[:], ALU.mult,
                           pG.d + mG.d, [Gs.d[h]])
                        ps_ = pS[h % 2]
                        mm(ps_[:, 0, :], AT, BT, True, True, ad, ps_.d)
                        mm(ps_[:, 1, :], BT, AT, True, True, ad, ps_.d)
                        tt("dve", S0[:, h, 0:2, :], ps_[:, 0:2, :], mQ[:], ALU.mult, ps_.d + mQ.d, [S0.d[h]])
                        tt("pool", S0[:, h, 2, :], S0[:, h, 1, :], ident[:], ALU.add, [S0.d[h]] + ident.d, [S0.d[h]])
                        mm(pG[0:64, 384 + h:385 + h], logw[:, hs], ones[:], True, True, logw.d + ones.d, pG.d)
                        act(WC[:, h:h + 1], pG[0:64, 384 + h:385 + h], AF.Exp, pG.d, [WC.d[h]])
                        drip(1)
                    for j in range(7):
                        Sc, Sn = Sb[j % 2], Sb[(j + 1) % 2]
                        for h in range(8):
                            ps_ = pS[h % 2]
                            Q, QT, PT = Sc[:, h, 0, :], Sc[:, h, 1, :], Sc[:, h, 2, :]
                            sd = [Sc.d[h]]
                            if j == 0:
                                mm(ps_[:, 0, :], QT, Q, True, True, sd, ps_.d)
                                mm(ps_[:, 1, :], Q, QT, True, True, sd, ps_.d)
                                cp("act", Sn[:, h, 0:2, :], ps_[:, 0:2, :], ps_.d, [Sn.d[h]])
                                cp("pool", Sn[:, h, 2, :], PT, sd, [Sn.d[h]])
                            elif j < 6:
                                mm(ps_[:, 0, :], QT, Q, True, True, sd, ps_.d)
                                mm(ps_[:, 1:3, :], Q, Sc[:, h, 1:3, :], True, True, sd, ps_.d)
                                cp("act", Sn[:, h, 0:2, :], ps_[:, 0:2, :], ps_.d, [Sn.d[h]])
                                tt("dve", Sn[:, h, 2, :], ps_[:, 2, :], PT, ALU.add, ps_.d + sd, [Sn.d[h]])
                            else:
                                mm(ps_[:, 2, :], Q, PT, True, True, sd, ps_.d)
                                tt("dve", Sn[:, h, 2, :], ps_[:, 2, :], PT, ALU.add, ps_.d + sd, [Sn.d[h]])
                            drip(1)
                    drip(1000)
                    Sf = Sb[1]
                    Zc, Zn = Z[b][c % 2], Z[b][(c + 1) % 2]
                    for h in range(8):
                        hs = slice(h * 64, (h + 1) * 64)
                        AT = ART[:, h, 0, :]
                        mm(pX[:, h, :], AT, Zc[:, h, :], True, False, [ART.d[h], Zc.d[h]], pX.d)
                        mm(pX[:, h, :], Gs[:, h, 1, :], vmix[:, hs], False, True, [Gs.d[h]] + vmix.d, pX.d)
                        cp("act", Xs[:, h, :], pX[:, h, :], pX.d, [Xs.d[h]])
                    for h in range(8):
                        mm(pX[:, h, :], Sf[:, h, 2, :], Xs[:, h, :], True, True, [Sf.d[h], Xs.d[h]], pX.d)
                        cp("dve", Us[:, h, :], pX[:, h, :], pX.d, [Us.d[h]])
                    for h in range(8):
                        hs = slice(h * 64, (h + 1) * 64)
                        RT = ART[:, h, 1, :]
                        mm(pY[:, hs], RT, Zc[:, h, :], True, False, [ART.d[h], Zc.d[h]], pY.d)
                        mm(pY[:, hs], Gs[:, h, 0, :], Us[:, h, :], False, False, [Gs.d[h], Us.d[h]], pY.d)
                        mm(pY[:, hs], Gs[:, h, 2, :], vmix[:, hs], False, True, [Gs.d[h]] + vmix.d, pY.d)
                        mm(pZ[:, h, 0:64], ARBK[:, 2, hs], Us[:, h, :], True, False, ARBK.d + [Us.d[h]], pZ.d)
                        mm(pZ[:, h, 0:64], ARBK[:, 3, hs], vmix[:, hs], False, False, ARBK.d + vmix.d, pZ.d)
                        mm(pZ[:, h, 0:64], ident[0:64, 0:64], Zc[:, h, :], False, True, ident.d + [Zc.d[h]], pZ.d)
                        act(Zn[:, h, :], pZ[:, h, 0:64], AF.Identity, pZ.d + [WC.d[h]], [Zn.d[h]], scale=WC[:, h:h + 1])
                    cp("act", y[:], pY[:], pY.d, y.d)
                    red("dve", s8[:], h3(y[:]), y.d, s8.d)
                    ts("dve", s8[:], s8[:], 1.0 / 64, None, ALU.mult, None, s8.d, s8.d)
                    tt("dve", h3(yc[:]), h3(y[:]), b8(s8[:]), ALU.subtract, y.d + s8.d, yc.d)
                    tt("pool", t6[:], yc[:], yc[:], ALU.mult, yc.d, t6.d)
                    red("dve", s8b[:], h3(t6[:]), t6.d, s8b.d)
                    act(s8b[:], s8b[:], AF.Sqrt, s8b.d, s8b.d, bias=64e-5, scale=1.0 / 64)
                    recip(s8b[:], s8b[:], s8b.d, s8b.d)
                    tt("dve", h3(yc[:]), h3(yc[:]), b8(s8b[:]), ALU.mult, yc.d + s8b.d, yc.d)
                    tt("pool", yc[:], yc[:], LNW, ALU.mult, yc.d + crow.d, yc.d)
                    tt("pool", yc[:], yc[:], LNB, ALU.add, yc.d + crow.d, yc.d)
                    tt("pool", yc[:], yc[:], bonus[:], ALU.add, yc.d + bonus.d, yc.d)
                    ro = rwo[k_]
                    tt("dve", ro[:], yc[:], lo_[:, 1024:1536], ALU.mult, yc.d + lo_.d, ro.d)
                    S.dma("sp", rwo_s[n0:n0 + 128, :], ro[:], reads=ro.d)

                items = [(b, c) for c in range(T // 128) for b in range(NB)]
                if lim is not None:
                    items = items[:lim]
                def mkdrip(g):
                    def drip(n):
                        if g is None:
                            return
                        for _ in range(n):
                            try:
                                next(g)
                            except StopIteration:
                                return
                    return drip

                loadB(0, *items[0])
                mkdrip(pre_gen(0, *items[0]))(1000)
                for idx, (b, c) in enumerate(items):
                    g = None
                    if idx + 1 < len(items):
                        loadB(idx + 1, *items[idx + 1])
                        g = pre_gen(idx + 1, *items[idx + 1])
                    chunk(idx, b, c, mkdrip(g))
                S.barrier()
                S.emit()

        def phaseC():
            with ExitStack() as ph:
                P = Pool_(ph)
                bias = P.sb("bias", [128, 3, 4, 2, 128])
                S.dma("sp", bias[:], biasT_d[:, :, :, :, :], writes=bias.d)
                mask = P.sb("mask", [128, 2, 128])
                S.dma("sp", mask[:], maskT_d[:, :, :], writes=mask.d)
                mneg = P.sb("mneg", [128, 2, 128])
                ts("dve", mneg[:], mask[:], -NEG, NEG, ALU.mult, ALU.add, mask.d, mneg.d)
                for g in range(3):
                    for h in range(4):
                        tt("dve", bias[:, g, h, :, :], bias[:, g, h, :, :], mask[:], ALU.mult, bias.d + mask.d, bias.d)
                        tt("dve", bias[:, g, h, :, :], bias[:, g, h, :, :], mneg[:], ALU.add, bias.d + mneg.d, bias.d)
                gains = P.sb("gains", [128, 512])
                S.dma("sp", gains[:], qkg_d.partition_broadcast(128), writes=gains.d)
                ts("dve", gains[:, 0:256], gains[:, 0:256], 0.125, None, ALU.mult, None, gains.d, gains.d)

                qkv = P.ring("qkv", [128, 3, 256], k=2)
                sq = P.sb("sq", [128, 512])
                s8 = P.sb("as8", [128, 8])
                qkn = P.sb("qkn", [128, 512])
                qkb = P.sb("qkb", [128, 512], BF16)
                vaug = P.ring("vaug", [128, 4, 65], BF16, k=3)
                for v_ in vaug:
                    mset("pool", v_[:, :, 64:65], 1.0, v_.d)
                ptq = P.ps("ptq", [128, 8, 128], BF16)
                qTr = P.ring("qT", [128, 2, 128], BF16, k=2)
                kT = P.ring("kT", [128, 2, 128], BF16, k=3)
                pst = [[P.ps(f"pst{w_}{hf}", [128, 4, 128]) for hf in range(2)] for w_ in range(2)]
                scb = P.ring("scb", [128, 4, 128], k=2)
                pTb = P.ring("pTb", [128, 4, 128], BF16, k=2)
                po_ = P.ps("po", [128, 512])
                po = po_[:, 0:260].rearrange("p (h d) -> p h d", h=4)
                ob = P.ring("ob", [128, 260], k=2)
                h8 = lambda ap: ap.rearrange("p (h d) -> p h d", h=8)
                items = []
                for b in range(NB):
                    for g, (win, dil) in enumerate(ATTN_GROUPS):
                        for z in range(dil):
                            for n in range(T // dil // 128):
                                items.append((b, g, dil, z, n))

                def loadC(idx, b, g, dil, z, n):
                    L = T // dil
                    pview = proj_s.rearrange("(l dd) c -> dd l c", dd=dil)
                    l0 = b * L + n * 128
                    q_ = qkv[idx % 2]
                    src = pview[z, l0:l0 + 128, 1536:3840].rearrange("l (w c) -> l w c", w=3)[:, :, g * 256:(g + 1) * 256]
                    S.dma("sp", q_[:], src, writes=q_.d)

                def c_stage1(idx, b, g, dil, z, n):
                    q_ = qkv[idx % 2]
                    qk2 = q_[:, 0:2, :]
                    tt("pool", sq[:].rearrange("p (w c) -> p w c", w=2), qk2, qk2, ALU.mult, q_.d, sq.d)
                    red("dve", s8[:], h8(sq[:]), sq.d, s8.d)
                    act(s8[:], s8[:], AF.Sqrt, s8.d, s8.d, bias=1e-6, scale=1.0 / 64)
                    recip(s8[:], s8[:], s8.d, s8.d)
                    tt("dve", h8(qkn[:]), qk2.rearrange("p w (h d) -> p (w h) d", h=4),
                       s8[:].unsqueeze(2).to_broadcast([128, 8, 64]), ALU.mult, q_.d + s8.d, qkn.d)
                    tt("pool", qkb[:], qkn[:], gains[:], ALU.mult, qkn.d + gains.d, qkb.d)
                    va = vaug[idx % 3]
                    cp("act", va[:, :, 0:64], q_[:, 2, :].rearrange("p (h d) -> p h d", h=4), q_.d, va.d)
                    for j in range(4):
                        tr(ptq[:, j, :], qkb[:, j * 128:(j + 1) * 128], identb[:], qkb.d + identb.d, ptq.d)
                    cp("act", qTr[idx % 2][:], ptq[:, 0:2, :], ptq.d, qTr[idx % 2].d)
                    cp("dve", kT[idx % 3][:], ptq[:, 2:4, :], ptq.d, kT[idx % 3].d)

                def c_stage2(idx, b, g, dil, z, n):
                    L = T // dil
                    aview = ao_s[g].rearrange("(l dd) c -> dd l c", dd=dil)
                    l0 = b * L + n * 128
                    qT = qTr[idx % 2]
                    kc_, kp_ = kT[idx % 3], kT[(idx - 1) % 3]
                    va, vp = vaug[idx % 3], vaug[(idx - 1) % 3]
                    blocks = [(1, kc_, va)] + ([(0, kp_, vp)] if n > 0 else [])
                    for (which, kt, _v) in blocks:
                        for h in range(4):
                            hp = slice((h % 2) * 64, (h % 2) * 64 + 64)
                            ps_ = pst[which][h % 2]
                            mm(ps_[:, h // 2, :], kt[hp, h // 2, :], qT[hp, h // 2, :], True, True,
                               kt.d + qT.d, ps_.d)
                        sb_ = scb[which]
                        for hf in range(2):
                            ps_ = pst[which][hf]
                            tt("dve", sb_[:].rearrange("p (pp two) q -> p two pp q", two=2)[:, hf, :, :], ps_[:, 0:2, :],
                               bias[:, g, :, :, :].rearrange("p (pp two) w q -> p two pp w q", two=2)[:, hf, :, which, :],
                               ALU.add, ps_.d + bias.d, sb_.d)
                        act(pTb[which][:], sb_[:], AF.Exp, sb_.d, pTb[which].d)
                    for h in range(4):
                        for bi, (which, _kt, v_) in enumerate(blocks):
                            mm(po[:, h, :], pTb[which][:, h, :], v_[:, h, :], bi == 0, bi == len(blocks) - 1,
                               pTb[which].d + v_.d, po_.d)
                    o_ = ob[idx % 2]
                    cp("act", o_[:], po_[:, 0:260], po_.d, o_.d)
                    S.dma("sp", aview[z, l0:l0 + 128, :], o_[:], reads=o_.d)

                if lim is not None:
                    items = items[:lim]
                NI = len(items)
                loadC(0, *items[0])
                if NI > 1:
                    loadC(1, *items[1])
                c_stage1(0, *items[0])
                for idx, itm in enumerate(items):
                    if idx + 2 < NI:
                        loadC(idx + 2, *items[idx + 2])
                    if idx + 1 < NI:
                        c_stage1(idx + 1, *items[idx + 1])
                    c_stage2(idx, *itm)
                S.barrier()
                S.emit()

        def phaseD():
            with ExitStack() as ph:
                P = Pool_(ph)
                wrr = P.sb("wrr", [128, 4, D], BF16)
                wra = P.sb("wra", [128, 2, D], BF16)
                wo = P.sb("wo", [128, 8, D], BF16)
                S.dma("pool", wrr[:], wbrr_d.rearrange("(c p) n -> p c n", p=128), writes=wrr.d)
                S.dma("pool", wra[:], wbra_d.rearrange("(c p) n -> p c n", p=128), writes=wra.d)
                S.dma("pool", wo[:], wout_d.rearrange("(c p) n -> p c n", p=128), writes=wo.d)
                rwf = P.sb("rwf", [128, 8, 32])
                S.dma("sp", rwf[:], rw_d.rearrange("(c p) n -> p c n", p=128), writes=rwf.d)
                rbb = P.sb("rbb", [128, 32])
                S.dma("sp", rbb[:], rb_d.partition_broadcast(128), writes=rbb.d)
                g1b = P.sb("g1b", [128, D])

                xt = P.ring("dxt", [128, D], k=3)
                gg = P.ring("gg", [128, 2048], k=2)
                rwt = P.ring("rwt", [128, 512], k=2)
                a3 = P.ring("a3", [128, 3, 260], k=2)
                asum = P.sb("asum", [128, 4, 65])
                rden = P.sb("rden", [128, 4])
                brb = P.sb("brb", [128, 768], BF16)
                ptb = P.ps("ptb", [128, 8, 128], BF16)
                brT = P.sb("brT", [128, 6, 128], BF16)
                py = P.ring("py", [128, 512], k=2, psum=True)
                sg = P.sb("sgate", [128, 2048])
                m1 = P.sb("m1", [128, D])
                m2 = P.sb("m2", [128, D])
                mxbr = P.ring("mxb", [128, D], BF16, k=2)
                mT = P.sb("mT", [128, 8, 128], BF16)
                x1 = P.ring("x1t", [128, D], k=2)
                junk = P.sb("djunk", [128, D])
                ss = P.sb("dss", [128, 1])
                xn = P.sb("dxn", [128, D])
                ptr = P.ps("dptr", [128, 8, 128], n=2)
                h2f = P.sb("h2f", [128, 8, 128], n=8)
                h2b = P.ring("h2b", [128, 8, 128], BF16, k=2)
                psm = P.ps("psm", [128, 512])
                lg = P.sb("lg", [128, 32])
                top8 = P.sb("top8", [128, 8])
                nmx = P.sb("nmx", [128, 1])
                msk = P.sb("msk", [128, 32])
                ex = P.sb("ex", [128, 32])
                ssum = P.sb("ssum", [128, 1])
                Gm_r = P.ring("Gm", [128, 32], k=2)
                GTs = P.ring("GTs", [32, 128], k=2)

                def loadD(i):
                    k_ = i % 2
                    n0 = i * 128
                    x_, g_, r_, a_ = xt[i % 3], gg[k_], rwt[k_], a3[k_]
                    S.dma("sp", x_[:], x_d[n0:n0 + 128, :], writes=x_.d)
                    S.dma("sp", g_[:], proj_s[n0:n0 + 128, 3840:5888], writes=g_.d)
                    S.dma("sp", r_[:], rwo_s[n0:n0 + 128, :], writes=r_.d)
                    for g in range(3):
                        S.dma("sp", a_[:, g, :], ao_s[g, n0:n0 + 128, :], writes=a_.d)

                def D_a(i):
                    k_ = i % 2
                    g_, r_, a_ = gg[k_], rwt[k_], a3[k_]
                    mxb = mxbr[k_]
                    a4 = lambda g: a_[:, g, :].rearrange("p (h d) -> p h d", h=4)
                    tt("dve", asum[:], a4(0), a4(1), ALU.add, a_.d, asum.d)
                    tt("dve", asum[:], asum[:], a4(2), ALU.add, asum.d + a_.d, asum.d)
                    recip(rden[:], asum[:, :, 64], asum.d, rden.d)
                    tt("dve", brb[:, 512:768].rearrange("p (h d) -> p h d", h=4), asum[:, :, 0:64],
                       rden[:].unsqueeze(2).to_broadcast([128, 4, 64]), ALU.mult, asum.d + rden.d, brb.d)
                    cp("pool", brb[:, 0:512], r_[:], r_.d, brb.d)
                    for c in range(6):
                        tr(ptb[:, c, :], brb[:, c * 128:(c + 1) * 128], identb[:], brb.d + identb.d, ptb.d)
                    cp("act", brT[:], ptb[:, 0:6, :], ptb.d, brT.d)
                    act(sg[:], g_[:], AF.Sigmoid, g_.d, sg.d)
                    for (c0, c1, wsrc, dst, so) in ((0, 4, wrr, m1, 0), (4, 6, wra, m2, 1024)):
                        for n2 in range(2):
                            p_ = py[n2]
                            for c in range(c0, c1):
                                mm(p_[:], brT[:, c, :], wsrc[:, c - c0, n2 * 512:(n2 + 1) * 512], c == c0, c == c1 - 1,
                                   brT.d + wsrc.d, p_.d)
                            tt("dve", dst[:, n2 * 512:(n2 + 1) * 512], p_[:], sg[:, so + n2 * 512:so + (n2 + 1) * 512],
                               ALU.mult, p_.d + sg.d, dst.d)
                    tt("pool", mxb[:], m1[:], m2[:], ALU.add, m1.d + m2.d, mxb.d)
                def D_b(i):
                    b = i // 16
                    k_ = i % 2
                    n0 = i * 128
                    x_ = xt[i % 3]
                    mxb = mxbr[k_]
                    for c in range(8):
                        tr(ptb[:, c, :], mxb[:, c * 128:(c + 1) * 128], identb[:], mxb.d + identb.d, ptb.d)
                    cp("act", mT[:], ptb[:], ptb.d, mT.d)
                    x1_ = x1[k_]
                    for n2 in range(2):
                        p_ = py[n2]
                        for c in range(8):
                            mm(p_[:], mT[:, c, :], wo[:, c, n2 * 512:(n2 + 1) * 512], c == 0, c == 7, mT.d + wo.d, p_.d)
                        tt("dve", x1_[:, n2 * 512:(n2 + 1) * 512], p_[:], g1b[:, n2 * 512:(n2 + 1) * 512], ALU.mult,
                           p_.d + g1b.d, x1_.d)
                    tt("pool", x1_[:], x1_[:], x_[:], ALU.add, x1_.d + x_.d, x1_.d)
                    S.dma("sp", x1_s[n0:n0 + 128, :], x1_[:], reads=x1_.d)
                    act(junk[:], x1_[:], AF.Square, x1_.d, junk.d + ss.d, accum_out=ss[:])
                    act(ss[:], ss[:], AF.Sqrt, ss.d, ss.d, bias=1e-6, scale=1.0 / D)
                    recip(ss[:], ss[:], ss.d, ss.d)
                    ts("dve", xn[:], x1_[:], ss[:, 0:1], None, ALU.mult, None, x1_.d + ss.d, xn.d)
                    for c in range(8):
                        tr(ptr[:, c, :], xn[:, c * 128:(c + 1) * 128], ident[:], xn.d + ident.d, [ptr.d[c // 4]])
                    for c in range(8):
                        if c < 4:
                            act(h2f[:, c, :], ptr[:, c, :], AF.Identity, [ptr.d[c // 4]] + sc2.d + sh2.d, [h2f.d[c]],
                                bias=sh2[:, c, b:b + 1], scale=sc2[:, c, b:b + 1])
                        else:
                            ts("dve", h2f[:, c, :], ptr[:, c, :], sc2[:, c, b:b + 1], sh2[:, c, b:b + 1],
                               ALU.mult, ALU.add, [ptr.d[c // 4]] + sc2.d + sh2.d, [h2f.d[c]])
                    hb_ = h2b[k_]
                    cp("pool", hb_[:], h2f[:], h2f.d, hb_.d)
                    S.dma("sp", h2T_s[:, :, n0:n0 + 128], hb_[:], reads=hb_.d)
                    for c in range(8):
                        mm(psm[:, 0:32], h2f[:, c, :], rwf[:, c, :], c == 0, c == 7, h2f.d + rwf.d, psm.d)
                    tt("dve", lg[:], psm[:, 0:32], rbb[:], ALU.add, psm.d + rbb.d, lg.d)
                    S.op("dve", lambda e, o=top8, l=lg: e.max(out=o[:], in_=l[:]), lg.d, top8.d)
                    ts("dve", nmx[:], top8[:, 0:1], -1.0, None, ALU.mult, None, top8.d, nmx.d)
                    ts("dve", msk[:], lg[:], top8[:, 3:4], None, ALU.is_ge, None, lg.d + top8.d, msk.d)
                    act(ex[:], lg[:], AF.Exp, lg.d + nmx.d, ex.d, bias=nmx[:, 0:1], scale=1.0)
                    tt("dve", ex[:], ex[:], msk[:], ALU.mult, ex.d + msk.d, ex.d)
                    red("dve", ssum[:], ex[:], ex.d, ssum.d)
                    recip(ssum[:], ssum[:], ssum.d, ssum.d)
                    Gm = Gm_r[k_]
                    ts("dve", Gm[:], ex[:], ssum[:, 0:1], None, ALU.mult, None, ex.d + ssum.d, Gm.d)
                    S.dma("sp", Gtm_s[n0:n0 + 128, :], Gm[:], reads=Gm.d)
                    tr(psm[0:32, 128:256], Gm[:], ident[:], Gm.d + ident.d, psm.d)
                    gt_ = GTs[k_]
                    cp("act", gt_[:], psm[0:32, 128:256], psm.d, gt_.d)
                    S.dma("sp", GT_s[:, n0:n0 + 128], gt_[:], reads=gt_.d)

                loadD(0)
                if NTL > 1:
                    loadD(1)
                D_a(0)
                for i in range(NTL):
                    if i % 16 == 0:
                        S.dma("sp", g1b[:], modrow_s[i // 16:i // 16 + 1, 2048:3072].partition_broadcast(128), writes=g1b.d)
                    if i + 2 < NTL:
                        loadD(i + 2)
                    if i + 1 < NTL:
                        D_a(i + 1)
                    D_b(i)
                S.barrier()
                S.emit()

        def phaseE():
            TB = 1024
            with ExitStack() as ph:
                P = Pool_(ph)
                b2 = P.sb("b2", [32, D])
                S.dma("sp", b2[:], eb2_d[:, :], writes=b2.d)
                b1 = P.sb("b1", [128, 32, 2, 8])
                S.dma("sp", b1[:], b1fm_d[:, :, :, :], writes=b1.d)
                g2b = P.sb("g2b", [128, D])
                h2 = P.sb("h2", [128, 8, TB], BF16)
                GT = P.sb("GTb", [32, TB])
                acc = P.sb("acc", [128, TB // 128, D], n=TB // 128)
                w1 = P.ring("w1_", [128, 4, 2 * D], BF16, k=3)
                w2 = P.ring("w2_", [128, 8, D], BF16, k=2)
                Gt = P.sb("Gt", [128, TB // 128, 32])
                ts("dve", b1[:, :, 1, :], b1[:, :, 1, :], 1.0, None, ALU.add, None, b1.d, b1.d)
                X1E = "dve"
                X2E = "dve"
                gp = P.ring("gp", [128, 512], k=2)
                sgm = P.ring("sgm", [128, 512], k=2)
                lp = P.ring("lp", [128, 512], k=2)
                actT = [P.sb(f"actT{k}", [128, 8, 512], BF16, n=8) for k in range(2)]
                pu = P.ring("pu", [128, 512], k=4, psum=True)
                po = P.ring("pmo", [128, 512], k=4, psum=True)
                xo = P.ring("xo", [128, D], k=2)
                x1t = P.ring("ex1", [128, D], k=2)
                def load_w1(ge, half):
                    e = ge % 32
                    u = w1[(2 * ge + half) % 3]
                    for kk_ in range(4):
                        kc = half * 4 + kk_
                        S.dma("pool", u[:, kk_, :], ew1_d[e, kc * 128:(kc + 1) * 128, :], writes=u.d)

                def load_w2(ge):
                    e = ge % 32
                    w2_ = w2[ge % 2]
                    S.dma("pool", w2_[:], ew2_d[e].rearrange("(c p) n -> p c n", p=128), writes=w2_.d)

                NTB = NTOK // TB if lim is None else min(NTOK // TB, lim)
                NGE = NTB * 32
                load_w1(0, 0)
                load_w1(0, 1)
                load_w2(0)

                for tb in range(NTB):
                    t0 = tb * TB
                    b = t0 // T
                    S.dma("sp", g2b[:], modrow_s[b:b + 1, 5120:6144].partition_broadcast(128), writes=g2b.d)
                    S.dma("sp", h2[:], h2T_s[:, :, t0:t0 + TB], writes=h2.d)
                    S.dma("sp", GT[:], GT_s[:, t0:t0 + TB], writes=GT.d)
                    S.dma("sp", Gt[:], Gtm_s[t0:t0 + TB, :].rearrange("(m p) e -> p m e", p=128), writes=Gt.d)
                    for m in range(TB // 128):
                        for n2 in range(2):
                            p_ = po[(m * 2 + n2) % 4]
                            mm(p_[:], GT[:, m * 128:(m + 1) * 128], b2[:, n2 * 512:(n2 + 1) * 512], True, True,
                               GT.d + b2.d, p_.d)
                            cp("act", acc[:, m, n2 * 512:(n2 + 1) * 512], p_[:], p_.d, [acc.d[m]])
                    for e in range(32):
                        ge = tb * 32 + e
                        u0, u1 = w1[(2 * ge) % 3], w1[(2 * ge + 1) % 3]
                        w2_ = w2[ge % 2]
                        if ge + 1 < NGE:
                            load_w1(ge + 1, 0)
                            load_w2(ge + 1)
                        for sbk in range(TB // 512):
                            tk = slice(sbk * 512, (sbk + 1) * 512)
                            aT = actT[sbk % 2]
                            for j in range(8):
                                pg_, pl_ = pu[(2 * j) % 4], pu[(2 * j + 1) % 4]
                                for (gl, p_) in ((0, pg_), (1, pl_)):
                                    for kc in range(8):
                                        u = u0 if kc < 4 else u1
                                        lhs = u[:, kc % 4, :].rearrange("p (j two) -> p two j", two=2)[:, gl, j * 128:(j + 1) * 128]
                                        mm(p_[:], lhs, h2[:, kc, tk], kc == 0, kc == 7, u.d + h2.d, p_.d)
                                g_, s_, l_ = gp[j % 2], sgm[j % 2], lp[j % 2]
                                ts("dve", g_[:], pg_[:], b1[:, e, 0, j:j + 1], 7.0, ALU.add, ALU.min, pg_.d + b1.d, g_.d)
                                act(s_[:], g_[:], AF.Sigmoid, g_.d, s_.d, scale=1.702)
                                ts("dve", l_[:], pl_[:], b1[:, e, 1, j:j + 1], 8.0, ALU.add, ALU.min, pl_.d + b1.d, l_.d)
                                stt(X1E, l_[:], l_[:], -6.0, g_[:], ALU.max, ALU.mult, l_.d + g_.d, l_.d)
                                tt(X2E, aT[:, j, :], l_[:], s_[:], ALU.mult, l_.d + s_.d, [aT.d[j]])
                            if sbk == TB // 512 - 1 and ge + 1 < NGE:
                                load_w1(ge + 1, 1)
                            for mi in range(4):
                                m = sbk * 4 + mi
                                for n2 in range(2):
                                    p_ = po[(mi * 2 + n2) % 4]
                                    for j in range(8):
                                        mm(p_[:], aT[:, j, mi * 128:(mi + 1) * 128], w2_[:, j, n2 * 512:(n2 + 1) * 512],
                                           j == 0, j == 7, [aT.d[j]] + w2_.d, p_.d)
                                    cs = slice(n2 * 512, (n2 + 1) * 512)
                                    stt("dve", acc[:, m, cs], p_[:], Gt[:, m, e:e + 1], acc[:, m, cs], ALU.mult, ALU.add,
                                        p_.d + Gt.d + [acc.d[m]], [acc.d[m]])
                    for m in range(TB // 128):
                        n0 = t0 + m * 128
                        x_, o_ = x1t[m % 2], xo[m % 2]
                        S.dma("sp", x_[:], x1_s[n0:n0 + 128, :], writes=x_.d)
                        tt("dve", o_[:], acc[:, m, :], g2b[:], ALU.mult, [acc.d[m]] + g2b.d, o_.d)
                        tt("pool", o_[:], o_[:], x_[:], ALU.add, o_.d + x_.d, o_.d)
                        S.dma("sp", out_d[n0:n0 + 128, :], o_[:], reads=o_.d)
                S.barrier()
                S.emit()

        nc.kernel_input_names = in_names
        for st_, fn in (("0", phase0), ("A", phaseA), ("B", phaseB), ("C", phaseC), ("D", phaseD), ("E", phaseE)):
            if st_ in stages:
                fn()
    return nc


def _t5_bucket(dist):
    max_exact = 16
    large = max_exact + (np.log(np.maximum(dist, max_exact).astype(np.float32) / max_exact)
                         / math.log(2048 / max_exact) * (32 - max_exact)).astype(np.int32)
    return np.where(dist < max_exact, dist, np.minimum(large, 31))


def _bias_index():
    qi = np.arange(128)[None, :]
    kj = np.arange(128)[:, None]
    idx = np.zeros((3, 128, 2, 128), np.int64)
    for g, (_w, dil) in enumerate(ATTN_GROUPS):
        for which in range(2):
            steps = qi - (kj + 128 * which) + 128
            idx[g, :, which, :] = _t5_bucket(np.maximum(steps, 0) * dil)
    return idx


def host_inputs(inp, NB=NBFULL, ncores=NCORES):
    f = lambda a: np.ascontiguousarray(a, dtype=np.float32)
    fm = lambda v: f(v.reshape(8, 128).T)
    L = 0
    crow = np.concatenate([inp["rwkv_mu_rkv"][L].reshape(-1), inp["rwkv_w0"][L], inp["rwkv_a0"][L], inp["rwkv_k_k"][L],
                           inp["rwkv_k_a"][L], inp["rwkv_r_k"][L].reshape(-1), inp["rwkv_ln_w"][L], inp["rwkv_ln_b"][L]])
    muwag = np.stack([fm(inp["rwkv_mu_wag"][L][k]) for k in range(3)], axis=1)
    lw1 = np.concatenate([inp["rwkv_w1"][L], inp["rwkv_a1"][L], inp["rwkv_g1"][L]], axis=1)
    l2 = np.zeros((128, 1536), np.float32)
    l2[0:64, 0:512] = inp["rwkv_w2"][L]
    l2[64:128, 512:1024] = inp["rwkv_a2"][L]
    l2[:, 1024:1536] = inp["rwkv_g2"][L]
    qkg = np.concatenate([np.tile(inp["attn_qn_g"][L], 4), np.tile(inp["attn_kn_g"][L], 4)])[None, :]
    idx = _bias_index()
    rb = inp["rel_bias"]
    biasT = np.zeros((128, 3, 4, 2, 128), np.float32)
    for g in range(3):
        for h in range(4):
            biasT[:, g, h, :, :] = rb[:, g * 4 + h][idx[g]]
    qi = np.arange(128)[None, :]
    kj = np.arange(128)[:, None]
    maskT = np.stack([(kj >= qi), (kj <= qi)], axis=1).astype(np.float32)
    tri = np.stack([(kj <= qi), (kj < qi), (qi < kj)], axis=1).astype(np.float32)
    b1 = inp["exp_b1"][L]
    b1fm = np.ascontiguousarray(b1.reshape(32, 8, 128, 2).transpose(2, 0, 3, 1))
    shared = dict(
        ada_w=f(inp["ada_w"][L]), ada_b=f(inp["ada_b"][L][None, :]), n1g=fm(inp["norm1_g"][L]), n2g=fm(inp["norm2_g"][L]),
        w_in=f(inp["w_in"][L]), crow=f(crow[None, :]), muwag=f(muwag), lw1=f(lw1), l2=l2, qkg=f(qkg),
        biasT=biasT, maskT=maskT, w_br_rwkv=f(inp["w_br_rwkv"][L]), w_br_attn=f(inp["w_br_attn"][L]),
        w_out=f(inp["w_out"][L]), router_w=f(inp["router_w"][L]), router_b=f(inp["router_b"][L][None, :]),
        exp_w1=f(inp["exp_w1"][L]), b1fm=f(b1fm), exp_w2=f(inp["exp_w2"][L]), exp_b2=f(inp["exp_b2"][L]),
        ident=np.eye(128, dtype=np.float32), tri=tri,
    )
    maps = []
    for core in range(ncores):
        xs = inp["x"][core * NB:(core + 1) * NB].reshape(NB * T, D)
        cs = inp["c"][core * NB:(core + 1) * NB]
        cT = np.ascontiguousarray(cs.T.reshape(8, 128, NB).transpose(1, 0, 2))
        m = dict(shared)
        m["x"] = f(xs)
        m["cT"] = f(cT)
        maps.append(m)
    return maps


def kernel(**inputs):
    inp = {k: np.asarray(v) for k, v in inputs.items()}
    nc = build()
    maps = host_inputs(inp)
    res = run_bass_kernel_spmd(nc, maps, core_ids=list(range(NCORES)))
    outs = [np.asarray(r["out"]).reshape(NBFULL, T, D) for r in res.results]
    return np.concatenate(outs, axis=0).astype(np.float32)
```
